# Optimizing a Trainium2 kernel written in Bass

```python
import math
import jax, jax.numpy as jnp
from jax import lax
import numpy as np

D_MODEL = 2048
BATCH = 2
SEQ = 8192
DEPTH = 4

N_MIXERS = 2
MEM_LEN = 256
MIX_W = D_MODEL
MEM_HEADS = 4
MEM_HEAD_DIM = 128
MEM_W = MEM_HEADS * MEM_HEAD_DIM
TOK_W = MIX_W - MEM_W
HEAD_DIM = 64
N_Q_HEADS = TOK_W // HEAD_DIM
N_KV_HEADS = 4
GQA_GROUP = N_Q_HEADS // N_KV_HEADS
KV_W = N_KV_HEADS * HEAD_DIM
WINDOW = 128
ATTN_BLOCK = WINDOW
A_IN_W = TOK_W + 2 * KV_W + MEM_W
CHUNK = 128
GM_GROUP_DIM = 128
GM_GROUPS = TOK_W // GM_GROUP_DIM
B_IN_W = 2 * TOK_W + MEM_W
N_GROUPS = 8
EXPERTS_PER_GROUP = 8
N_EXPERTS = N_GROUPS * EXPERTS_PER_GROUP
TOP_K = 2
D_EXPERT = 512
ROW_BLOCK = 128
ALPHA = (2 * DEPTH) ** 0.25
BETA = (8 * DEPTH) ** -0.25
LN_EPS = 1e-5
NEG_INF = -1e30

kernel_name = "hybrid_swa_sink_gmlp_memxattn_hmoe_deepnorm"


def alibi_slopes(n):
    def pow2(m):
        start = 2.0 ** (-8.0 / m)
        return [start ** (i + 1) for i in range(m)]
    if math.log2(n).is_integer():
        s = pow2(n)
    else:
        c = 2 ** math.floor(math.log2(n))
        s = pow2(c) + pow2(2 * c)[0::2][: n - c]
    return np.asarray(s, dtype=np.float32)


def layer_norm(x, g, b):
    xf = x.astype(jnp.float32)
    mu = jnp.mean(xf, axis=-1, keepdims=True)
    xc = xf - mu
    var = jnp.mean(xc * xc, axis=-1, keepdims=True)
    y = xc * lax.rsqrt(var + LN_EPS) * g.astype(jnp.float32) + b.astype(jnp.float32)
    return y.astype(x.dtype)


def sliding_window_sink_attention(q, k, v, sinks, slopes):
    B, S = q.shape[0], q.shape[1]
    nb = S // ATTN_BLOCK
    qb = q.reshape(B, nb, ATTN_BLOCK, N_KV_HEADS, GQA_GROUP, HEAD_DIM)
    kb = k.reshape(B, nb, ATTN_BLOCK, N_KV_HEADS, HEAD_DIM)
    vb = v.reshape(B, nb, ATTN_BLOCK, N_KV_HEADS, HEAD_DIM)
    pad = ((0, 0), (1, 0), (0, 0), (0, 0), (0, 0))
    kk = jnp.concatenate([jnp.pad(kb[:, :-1], pad), kb], axis=2)
    vv = jnp.concatenate([jnp.pad(vb[:, :-1], pad), vb], axis=2)
    scores = jnp.einsum('bnqkgd,bnskd->bnkgqs', qb, kk).astype(jnp.float32) * (HEAD_DIM ** -0.5)
    qi = jnp.arange(ATTN_BLOCK)[:, None]
    kj = jnp.arange(2 * ATTN_BLOCK)[None, :]
    dist = ATTN_BLOCK + qi - kj
    in_band = (dist >= 0) & (dist < WINDOW)
    has_prev = (jnp.arange(nb) > 0)[:, None, None] | (kj >= ATTN_BLOCK)[None]
    valid = in_band[None] & has_prev
    bias = -slopes.reshape(N_KV_HEADS, GQA_GROUP)[:, :, None, None] * dist.astype(jnp.float32)
    logits = jnp.where(valid[None, :, None, None], scores + bias, NEG_INF)
    sink = sinks.astype(jnp.float32).reshape(1, 1, N_KV_HEADS, GQA_GROUP, 1, 1)
    m = jnp.maximum(jnp.max(logits, axis=-1, keepdims=True), sink)
    p = jnp.exp(logits - m)
    probs = p / (jnp.sum(p, axis=-1, keepdims=True) + jnp.exp(sink - m))
    out = jnp.einsum('bnkgqs,bnskd->bnqkgd', probs.astype(v.dtype), vv)
    return out.reshape(B, S, N_Q_HEADS * HEAD_DIM)


def chunked_spatial_gating(z, norm_g, norm_b, w_s, b_s):
    B, S = z.shape[0], z.shape[1]
    z = jax.nn.gelu(z, approximate=False)
    u, v = z[..., :TOK_W], z[..., TOK_W:]
    v = layer_norm(v, norm_g, norm_b)
    v = v.reshape(B, S // CHUNK, CHUNK, GM_GROUPS, GM_GROUP_DIM)
    sv = jnp.einsum('gts,bcsgd->bctgd', jnp.tril(w_s), v) + b_s.T[:, :, None]
    return u * sv.reshape(B, S, TOK_W)


def memory_attention(qm, km, vm):
    B, S = qm.shape[0], qm.shape[1]
    s = jnp.einsum('bshd,bmhd->bhsm', qm, km).astype(jnp.float32) * (MEM_HEAD_DIM ** -0.5)
    p = jax.nn.softmax(s, axis=-1)
    out = jnp.einsum('bhsm,bmhd->bshd', p.astype(vm.dtype), vm)
    return out.reshape(B, S, MEM_W)


def hierarchical_moe(x2d, w_rg, b_rg, w_re, b_re, w_gate, w_up, w_down):
    T, D = x2d.shape
    g_logits = (x2d @ w_rg).astype(jnp.float32) + b_rg.astype(jnp.float32)
    g_prob = jax.nn.softmax(g_logits, axis=-1)
    g_sel = jnp.argmax(g_logits, axis=-1)
    p_grp = jnp.take_along_axis(g_prob, g_sel[:, None], axis=1)
    e_logits = ((x2d @ w_re).astype(jnp.float32) + b_re.astype(jnp.float32)).reshape(
        T, N_GROUPS, EXPERTS_PER_GROUP)
    e_logits = jnp.take_along_axis(e_logits, g_sel[:, None, None], axis=1)[:, 0]
    top_p, top_i = lax.top_k(jax.nn.softmax(e_logits, axis=-1), TOP_K)
    gate = p_grp * top_p / jnp.sum(top_p, axis=-1, keepdims=True)
    expert = g_sel[:, None].astype(jnp.int32) * EXPERTS_PER_GROUP + top_i.astype(jnp.int32)

    n_assign = T * TOP_K
    n_rows = -(-(n_assign + N_EXPERTS * (ROW_BLOCK - 1)) // ROW_BLOCK) * ROW_BLOCK
    e_flat = expert.reshape(-1)
    tok_flat = jnp.repeat(jnp.arange(T, dtype=jnp.int32), TOP_K)
    counts = jnp.bincount(e_flat, length=N_EXPERTS)
    padded = (counts + ROW_BLOCK - 1) // ROW_BLOCK * ROW_BLOCK
    start = jnp.cumsum(counts) - counts
    pstart = jnp.cumsum(padded) - padded
    order = jnp.argsort(e_flat)
    e_sorted = e_flat[order]
    dest = pstart[e_sorted] + jnp.arange(n_assign, dtype=jnp.int32) - start[e_sorted]
    row_tok = jnp.full((n_rows,), T, jnp.int32).at[dest].set(tok_flat[order])
    row_gate = jnp.zeros((n_rows,), jnp.float32).at[dest].set(gate.reshape(-1)[order])
    blk_start = jnp.arange(n_rows // ROW_BLOCK, dtype=jnp.int32) * ROW_BLOCK
    blk_expert = jnp.minimum(
        jnp.searchsorted(jnp.cumsum(padded), blk_start, side='right'), N_EXPERTS - 1)
    x_rows = jnp.concatenate([x2d, jnp.zeros((1, D), x2d.dtype)], axis=0)[row_tok]
    x_rows = x_rows.reshape(n_rows // ROW_BLOCK, ROW_BLOCK, D)

    def expert_block(args):
        xb, e = args
        h = jax.nn.silu(xb @ w_gate[e]) * (xb @ w_up[e])
        return h @ w_down[e]

    y_rows = lax.map(expert_block, (x_rows, blk_expert)).reshape(n_rows, D)
    y_rows = y_rows * row_gate[:, None].astype(y_rows.dtype)
    return jax.ops.segment_sum(y_rows, row_tok, num_segments=T + 1)[:T]


def setup_inputs(seed: int = 0) -> dict:
    key = jax.random.key(seed)
    ks = jax.random.split(key, 24)
    n_a = (DEPTH + 1) // 2
    n_b = DEPTH // 2
    f32 = jnp.float32

    def nrm(k, shape, fan_in, scale=1.0):
        return jax.random.normal(k, shape, f32) * (scale * fan_in ** -0.5)

    def near_one(k, shape):
        return 1.0 + 0.02 * jax.random.normal(k, shape, f32)

    def small(k, shape, s=0.02):
        return s * jax.random.normal(k, shape, f32)

    x = jax.random.normal(ks[0], (BATCH, SEQ, D_MODEL), f32)
    mem = jax.random.normal(ks[1], (BATCH, MEM_LEN, D_MODEL), f32)
    a_col = jnp.concatenate([jnp.ones((TOK_W + KV_W,), f32), jnp.full((KV_W,), BETA, f32),
                             jnp.ones((MEM_W,), f32)])
    a_w_in = nrm(ks[2], (n_a, D_MODEL, A_IN_W), D_MODEL) * a_col
    a_sinks = 0.5 * jax.random.normal(ks[3], (n_a, N_Q_HEADS), f32)
    b_w_in = nrm(ks[4], (n_b, D_MODEL, B_IN_W), D_MODEL)
    b_norm_g = near_one(ks[5], (n_b, TOK_W))
    b_norm_b = small(ks[6], (n_b, TOK_W))
    b_w_spatial = nrm(ks[7], (n_b, GM_GROUPS, CHUNK, CHUNK), CHUNK)
    b_b_spatial = near_one(ks[8], (n_b, GM_GROUPS, CHUNK))
    m_col = jnp.concatenate([jnp.ones((MEM_W,), f32), jnp.full((MEM_W,), BETA, f32)])
    w_mem_kv = nrm(ks[9], (DEPTH, D_MODEL, 2 * MEM_W), D_MODEL) * m_col
    w_out = nrm(ks[10], (DEPTH, MIX_W, D_MODEL), MIX_W, BETA)
    ln_g = near_one(ks[11], (DEPTH, 2, D_MODEL))
    ln_b = small(ks[12], (DEPTH, 2, D_MODEL))
    w_router_group = nrm(ks[13], (DEPTH, D_MODEL, N_GROUPS), D_MODEL)
    b_router_group = small(ks[14], (DEPTH, N_GROUPS), 0.01)
    w_router_expert = nrm(ks[15], (DEPTH, D_MODEL, N_EXPERTS), D_MODEL)
    b_router_expert = small(ks[16], (DEPTH, N_EXPERTS), 0.01)
    w_gate = nrm(ks[17], (DEPTH, N_EXPERTS, D_MODEL, D_EXPERT), D_MODEL)
    w_up = nrm(ks[18], (DEPTH, N_EXPERTS, D_MODEL, D_EXPERT), D_MODEL)
    w_down = nrm(ks[19], (DEPTH, N_EXPERTS, D_EXPERT, D_MODEL), D_EXPERT, BETA)
    return {"x": x, "mem": mem, "a_w_in": a_w_in, "a_sinks": a_sinks,
            "b_w_in": b_w_in, "b_norm_g": b_norm_g, "b_norm_b": b_norm_b,
            "b_w_spatial": b_w_spatial, "b_b_spatial": b_b_spatial,
            "w_mem_kv": w_mem_kv, "w_out": w_out, "ln_g": ln_g, "ln_b": ln_b,
            "w_router_group": w_router_group, "b_router_group": b_router_group,
            "w_router_expert": w_router_expert, "b_router_expert": b_router_expert,
            "w_gate": w_gate, "w_up": w_up, "w_down": w_down}


def reference(x, mem, a_w_in, a_sinks, b_w_in, b_norm_g, b_norm_b, b_w_spatial, b_b_spatial,
              w_mem_kv, w_out, ln_g, ln_b, w_router_group, b_router_group,
              w_router_expert, b_router_expert, w_gate, w_up, w_down):
    B, S, D = x.shape
    M = mem.shape[1]
    slopes = jnp.asarray(alibi_slopes(N_Q_HEADS))
    for i in range(DEPTH):
        j = i // N_MIXERS
        if i % N_MIXERS == 0:
            proj = x @ a_w_in[j]
            q = proj[..., :TOK_W].reshape(B, S, N_Q_HEADS, HEAD_DIM)
            k = proj[..., TOK_W:TOK_W + KV_W].reshape(B, S, N_KV_HEADS, HEAD_DIM)
            v = proj[..., TOK_W + KV_W:TOK_W + 2 * KV_W].reshape(B, S, N_KV_HEADS, HEAD_DIM)
            qm = proj[..., TOK_W + 2 * KV_W:]
            mix = sliding_window_sink_attention(q, k, v, a_sinks[j], slopes)
        else:
            proj = x @ b_w_in[j]
            qm = proj[..., 2 * TOK_W:]
            mix = chunked_spatial_gating(proj[..., :2 * TOK_W], b_norm_g[j], b_norm_b[j],
                                         b_w_spatial[j], b_b_spatial[j])
        kv_mem = mem @ w_mem_kv[i]
        mo = memory_attention(qm.reshape(B, S, MEM_HEADS, MEM_HEAD_DIM),
                              kv_mem[..., :MEM_W].reshape(B, M, MEM_HEADS, MEM_HEAD_DIM),
                              kv_mem[..., MEM_W:].reshape(B, M, MEM_HEADS, MEM_HEAD_DIM))
        y = jnp.concatenate([mix, mo], axis=-1) @ w_out[i]
        x = layer_norm(ALPHA * x + y, ln_g[i, 0], ln_b[i, 0])
        y = hierarchical_moe(x.reshape(B * S, D), w_router_group[i], b_router_group[i],
                             w_router_expert[i], b_router_expert[i],
                             w_gate[i], w_up[i], w_down[i]).reshape(B, S, D)
        x = layer_norm(ALPHA * x + y, ln_g[i, 1], ln_b[i, 1])
    return x
```

```python
import contextlib
import math
import numpy as np
import concourse.bass as bass
import concourse.mybir as mybir
from concourse.bass_utils import run_bass_kernel_spmd

F32 = mybir.dt.float32
BF16 = mybir.dt.bfloat16
I32 = mybir.dt.int32
AF = mybir.ActivationFunctionType
ALU = mybir.AluOpType
AX = mybir.AxisListType

ENGS = ["tensor", "vector", "scalar", "gpsimd", "sync"]

D = 2048
KC = 16
DEPTH = 4
TOK_W = 1536
MEM_W = 512
NQH = 24
NE = 64
DE = 512
ALPHA = (2 * DEPTH) ** 0.25
LN_EPS = 1e-5
NOWN = 32
CPB = 64 // NOWN
NCORES = 2 * CPB
NBLK = NOWN + 2
NTOK = NBLK * 128
CAP = 384
OOB = 0x3FFFFFFF
BIG = 30000.0


def alibi_slopes(n):
    def pow2(m):
        start = 2.0 ** (-8.0 / m)
        return [start ** (i + 1) for i in range(m)]
    if math.log2(n).is_integer():
        s = pow2(n)
    else:
        c = 2 ** math.floor(math.log2(n))
        s = pow2(c) + pow2(2 * c)[0::2][: n - c]
    return [float(np.float32(v)) for v in s]


SLOPES = alibi_slopes(NQH)


class T:
    __slots__ = ("w", "r", "name")

    def __init__(self, name=""):
        self.w = {}
        self.r = {}
        self.name = name


class Prog:
    def __init__(self, nc, stack, n_dma_sems=8, self_sync=True):
        self.nc = nc
        self.ops = {e: [] for e in ENGS}
        self.sem = {e: stack.enter_context(nc.semaphore("s_" + e)) for e in ENGS}
        self.cnt = {e: 0 for e in ENGS}
        self.known = {e: {} for e in ENGS}
        self.dpool = {}
        self.dnext = {}
        for q in ("sync", "scalar", "gpsimd"):
            nq = 5 if q == "gpsimd" else n_dma_sems
            self.dpool[q] = [[stack.enter_context(nc.semaphore("d_%s%d" % (q, i))), 0]
                             for i in range(nq)]
            self.dnext[q] = 0
        self.self_sync = self_sync
        self.ninst = 0
        self.reg_vals = []
        self.regs = {}

    def _wait(self, e, sem, v):
        if self.known[e].get(sem, 0) >= v:
            return
        self.known[e][sem] = v
        self.ops[e].append(lambda eng, sem=sem, v=v: eng.wait_ge(sem, v))
        self.ninst += 1

    def _waits(self, e, reads, writes, acc):
        deps = {}

        def add(s, v):
            if deps.get(s, 0) < v:
                deps[s] = v
        for t in reads:
            for s, v in t.w.items():
                add(s, v)
        for t in writes:
            for s, v in t.w.items():
                add(s, v)
            for s, v in t.r.items():
                add(s, v)
        for t in acc:
            for s, v in t.r.items():
                add(s, v)
        own = self.sem[e]
        for s, v in deps.items():
            if s is own and (e == "tensor" or not self.self_sync):
                continue
            self._wait(e, s, v)

    def _mark(self, sem, c, reads, writes, acc):
        for t in reads:
            t.r[sem] = c
        for t in writes:
            t.w = {sem: c}
            t.r = {}
        for t in acc:
            t.w[sem] = c

    def op(self, e, fn, reads=(), writes=(), acc=()):
        self._waits(e, reads, writes, acc)
        self.cnt[e] += 1
        c = self.cnt[e]
        sem = self.sem[e]
        self.ops[e].append(lambda eng, fn=fn, sem=sem: fn(eng).then_inc(sem, 1))
        self.ninst += 1
        self._mark(sem, c, reads, writes, acc)

    def dma(self, q, fns, reads=(), writes=(), acc=()):
        if not isinstance(fns, (list, tuple)):
            fns = [fns]
        pool = self.dpool[q]
        i = self.dnext[q]
        self.dnext[q] = (i + 1) % len(pool)
        sem, cur = pool[i]
        if cur > 0:
            self._wait(q, sem, cur)
        self._waits(q, reads, writes, acc)
        for fn in fns:
            cur += 16
            self.ops[q].append(lambda eng, fn=fn, sem=sem: fn(eng).then_inc(sem, 16))
            self.ninst += 1
        pool[i][1] = cur
        self._mark(sem, cur, reads, writes, acc)

    def barrier(self):
        for e in ENGS:
            for q in self.dpool:
                for sem, cur in self.dpool[q]:
                    if cur > 0:
                        self._wait(e, sem, cur)
            for e2 in ENGS:
                if e2 != e and self.cnt[e2] > 0:
                    self._wait(e, self.sem[e2], self.cnt[e2])

    def emit(self):
        ops = self.ops
        with self.nc.Block() as block:
            @block.sync
            def _(eng):
                for f in ops["sync"]:
                    f(eng)

            @block.tensor
            def _(eng):
                for f in ops["tensor"]:
                    f(eng)

            @block.vector
            def _(eng):
                for f in ops["vector"]:
                    f(eng)

            @block.scalar
            def _(eng):
                for f in ops["scalar"]:
                    f(eng)

            @block.gpsimd
            def _(eng):
                self.regs = {v: eng.to_reg(v) for v in self.reg_vals}
                for f in ops["gpsimd"]:
                    f(eng)
                for v, rg in self.regs.items():
                    eng.free_register(rg)
        self.ops = {e: [] for e in ENGS}


class Ring:
    def __init__(self, items):
        self.items = items
        self.i = 0

    def next(self):
        it = self.items[self.i]
        self.i = (self.i + 1) % len(self.items)
        return it


class WStream:
    def __init__(self, P, bufs, depth):
        self.P = P
        self.bufs = bufs
        self.depth = depth
        self.seq = []
        self.issued = 0

    def add(self, fn):
        self.seq.append(fn)
        return len(self.seq) - 1

    def _issue(self, k):
        buf, t = self.bufs[k % len(self.bufs)]
        self.P.dma("gpsimd", self.seq[k](buf), writes=[t])

    def get(self, k, depth=None):
        upto = min(len(self.seq) - 1, k + (self.depth if depth is None else depth))
        while self.issued <= upto:
            self._issue(self.issued)
            self.issued += 1
        return self.bufs[k % len(self.bufs)]


def build_program(plans, cap=CAP, dbg_stop=None):
    nc = bass.Bass("TRN2", target_bir_lowering=False)
    layers = [p["layer"] for p in plans]

    in_names = []
    _IN_NAMES[id(nc)] = in_names

    def din(name, shape, dt=F32):
        in_names.append(name)
        return nc.dram_tensor(name, list(shape), dt, kind="ExternalInput").ap()

    x_in = din("x_in", [NTOK, D])
    mem_in = din("mem", [256, D])
    W = {}
    for p in plans:
        L_ = p["layer"]
        sfx = "_%d" % L_
        w = {}
        if L_ % 2 == 0:
            w["w_in"] = din("a_w_in" + sfx, [D, 2560])
            w["sinks"] = din("a_sinks" + sfx, [NQH])
        else:
            w["w_in"] = din("b_w_in" + sfx, [D, 3584])
            w["norm_g"] = din("b_norm_g" + sfx, [TOK_W])
            w["norm_b"] = din("b_norm_b" + sfx, [TOK_W])
            w["w_sp"] = din("b_w_spatial" + sfx, [12, 128, 128])
            w["b_sp"] = din("b_b_spatial" + sfx, [12 * 128])
        w["w_mem_kv"] = din("w_mem_kv" + sfx, [D, 1024])
        w["w_out"] = din("w_out" + sfx, [D, D])
        w["ln_g"] = din("ln_g" + sfx, [2, D])
        w["ln_b"] = din("ln_b" + sfx, [2, D])
        w["w_rg"] = din("w_router_group" + sfx, [D, 8])
        w["b_rg"] = din("b_router_group" + sfx, [8])
        w["w_re"] = din("w_router_expert" + sfx, [D, NE])
        w["b_re"] = din("b_router_expert" + sfx, [NE])
        if dbg_stop is None:
            w["w_gate"] = din("w_gate" + sfx, [NE, D, DE])
            w["w_up"] = din("w_up" + sfx, [NE, D, DE])
            w["w_down"] = din("w_down" + sfx, [NE, DE, D])
        W[L_] = w
    c_ident = din("c_ident", [128, 128])
    c_dist = din("c_dist", [128, 256])
    c_dist_first = din("c_dist_first", [128, 256])
    c_triu = din("c_triu", [128, 128])
    c_lstrict = din("c_lstrict", [128, 128])
    c_iota = din("c_iota", [128, NE])
    c_tokidx = din("c_tokidx", [128, NBLK * 4], I32)
    c_oob = din("c_oob", [NE * cap + 256, 2], I32)
    c_pidx = din("c_pidx", [128, 2])
    y_out = nc.dram_tensor("y_out", [NOWN * 128, D], F32, kind="ExternalOutput").ap()

    xa = nc.dram_tensor("s_xa", [NTOK, D], F32, kind="Internal").ap()
    x1d = nc.dram_tensor("s_x1", [NTOK, D], F32, kind="Internal").ap()
    ybuf = nc.dram_tensor("s_ybuf", [NTOK * 2 + 128, D], F32, kind="Internal").ap()
    rowtab = nc.dram_tensor("s_rowtab", [NE * cap + 256, 2], I32, kind="Internal").ap()

    t_xa = [T("xa%d" % b) for b in range(NBLK)]
    t_x1 = [T("x1_%d" % b) for b in range(NBLK)]
    t_x1all = T("x1all")
    t_ybuf = T("ybuf")
    t_trash = T("trash")
    t_rowtab = T("rowtab")
    t_yout = T("yout")

    with contextlib.ExitStack() as top:
        P = Prog(nc, top)

        uid = [0]

        def sb(st, name, shape, dt):
            uid[0] += 1
            return st.enter_context(nc.sbuf_tensor("%s_%d" % (name, uid[0]), list(shape), dt))

        def ps(st, name, shape, dt):
            return st.enter_context(nc.psum_tensor(name, list(shape), dt))

        ident = sb(top, "ident", [128, 128], F32)
        identb = sb(top, "identb", [128, 128], BF16)
        dist = sb(top, "dist", [128, 256], F32)
        dist1 = sb(top, "dist1", [128, 256], F32)
        triu = sb(top, "triu", [128, 128], F32)
        lstrict = sb(top, "lstrict", [128, 128], BF16)
        onesb = sb(top, "onesb", [128, 128], BF16)
        iota = sb(top, "iota", [128, NE], F32)
        tokidx = sb(top, "tokidx", [128, NBLK * 4], I32)
        pidx = sb(top, "pidx", [128, 2], F32)
        gates = sb(top, "gates", [128, NBLK * 2], F32)
        tmpc = sb(top, "tmpc", [128, 128], F32)
        t_const = T("const")
        t_gates = [T("gates%d" % b) for b in range(NBLK)]
        t_tmpc = T("tmpc")

        P.dma("sync", [lambda e: e.dma_start(out=ident[:], in_=c_ident),
                       lambda e: e.dma_start(out=dist[:], in_=c_dist),
                       lambda e: e.dma_start(out=dist1[:], in_=c_dist_first),
                       lambda e: e.dma_start(out=triu[:], in_=c_triu),
                       lambda e: e.dma_start(out=iota[:], in_=c_iota),
                       lambda e: e.dma_start(out=tokidx[:], in_=c_tokidx),
                       lambda e: e.dma_start(out=pidx[:], in_=c_pidx),
                       lambda e: e.dma_start(out=tmpc[:], in_=c_lstrict)],
              writes=[t_const, t_tmpc])
        P.op("vector", lambda e: e.tensor_copy(out=identb[:], in_=ident[:]), reads=[t_const], writes=[T()])
        P.op("vector", lambda e: e.tensor_copy(out=lstrict[:], in_=tmpc[:]), reads=[t_tmpc], writes=[T()])
        P.op("vector", lambda e: e.memset(onesb[:], 1.0), writes=[T()])
        P.barrier()
        P.emit()

        psf_t = [ps(top, "psf%d" % i, [128, 512], F32) for i in range(6)]
        psb_t = [ps(top, "psb%d" % i, [128, 1024], BF16) for i in range(2)]
        psf = Ring([(t, T("psf%d" % i)) for i, t in enumerate(psf_t)])
        psb = Ring([(t, T("psb%d" % i)) for i, t in enumerate(psb_t)])

        evac_tog = [0]

        def evac(out_ap, in_ap, reads, writes=(), acc=()):
            evac_tog[0] ^= 1
            if evac_tog[0]:
                P.op("vector", lambda e: e.tensor_copy(out=out_ap, in_=in_ap), reads=reads, writes=writes, acc=acc)
            else:
                P.op("scalar", lambda e: e.activation(out=out_ap, in_=in_ap, func=AF.Copy),
                     reads=reads, writes=writes, acc=acc)

        def wchunk_dma(src_rows_cols):
            def fn(buf):
                src = src_rows_cols.rearrange("(kc p) n -> p kc n", p=128)
                return [lambda e: e.dma_start(out=buf[:, :, :], in_=src)]
            return fn

        def wdown_dma(src):
            def fn(buf):
                s = src.rearrange("(fc p) n -> p fc n", p=128)
                dst = buf[:].rearrange("p a b -> p (a b)").rearrange("p (fc n) -> p fc n", fc=4)
                return [lambda e: e.dma_start(out=dst, in_=s)]
            return fn

        def layer_norm_rows(st_sc, r, t_r, gbc, bbc, t_gb, out_ap, t_out, tagi):
            stats, mv, sc, t_s, t_mv, t_sc = st_sc
            for c in range(4):
                P.op("vector", lambda e, c=c: e.bn_stats(out=stats[:, c, :], in_=r[:, c * 512:(c + 1) * 512]),
                     reads=[t_r], acc=[t_s])
            P.op("vector", lambda e: e.bn_aggr(out=mv[:, 0:2], in_=stats[:].rearrange("p a b -> p (a b)")),
                 reads=[t_s], writes=[t_mv])
            P.op("vector", lambda e: e.tensor_scalar(out=sc[:, 0:1], in0=mv[:, 1:2], scalar1=LN_EPS, scalar2=None,
                                                     op0=ALU.add), reads=[t_mv], writes=[t_sc])
            P.op("scalar", lambda e: e.activation(out=sc[:, 1:2], in_=sc[:, 0:1], func=AF.Ln),
                 reads=[t_sc], writes=[t_sc])
            P.op("scalar", lambda e: e.activation(out=sc[:, 2:3], in_=sc[:, 1:2], func=AF.Exp, scale=-0.5),
                 reads=[t_sc], writes=[t_sc])
            P.op("vector", lambda e: e.scalar_tensor_tensor(out=sc[:, 3:4], in0=mv[:, 0:1], scalar=-1.0,
                                                            in1=sc[:, 2:3], op0=ALU.mult, op1=ALU.mult),
                 reads=[t_mv, t_sc], writes=[t_sc])
            P.op("scalar", lambda e: e.activation(out=r[:], in_=r[:], func=AF.Identity,
                                                  bias=sc[:, 3:4], scale=sc[:, 2:3]),
                 reads=[t_sc, t_r], writes=[t_r])
            P.op("vector", lambda e: e.tensor_tensor(out=r[:], in0=r[:], in1=gbc[:], op=ALU.mult),
                 reads=[t_r, t_gb], writes=[t_r])
            P.op("vector", lambda e: e.tensor_tensor(out=out_ap, in0=r[:], in1=bbc[:], op=ALU.add),
                 reads=[t_r, t_gb], writes=[t_out])

        for pi, plan in enumerate(plans):
            L = plan["layer"]
            j = L // 2
            is_attn = (L % 2 == 0)
            inblk = plan["inblk"]
            fullblk = plan["fullblk"]
            first_layer = (pi == 0)
            src_x = x_in if first_layer else xa
            t_src = [T() for _ in range(NBLK)] if first_layer else t_xa
            last_layer = (pi == len(plans) - 1)

            with contextlib.ExitStack() as st:
                wb = [(sb(st, "wb%d" % i, [128, KC, 512], BF16), T("wb%d" % i)) for i in range(4)]
                ws = WStream(P, wb, depth=1)
                xt = sb(st, "xt", [128, D], F32)
                t_xt = T("xt")
                xT = sb(st, "xT", [128, KC, 512], BF16)
                t_xT = T("xT")
                qmT = sb(st, "qmT", [128, 4, 512], BF16)
                t_qmT = T("qmT")
                mo = 0 if is_attn else 1536
                mix = sb(st, "mix", [128, D - mo], BF16)
                t_mix = T("mix")
                mixT = sb(st, "mixT", [128, KC, 512], BF16) if not is_attn else sb(st, "mixT", [128, KC, 128], BF16)
                t_mixT = T("mixT")
                r = sb(st, "r", [128, D], F32)
                t_r = T("r")
                x1T = sb(st, "x1T", [128, KC, 128], F32)
                t_x1T = T("x1T")
                lng = sb(st, "lng", [128, D], F32)
                lnb = sb(st, "lnb", [128, D], F32)
                t_ln = T("ln")
                kmT = sb(st, "kmT", [128, 4, 256], BF16)
                vm = sb(st, "vm", [128, 2, 512], BF16)
                t_km = T("km")
                Wr = sb(st, "Wr", [128, KC, 72], F32)
                rbc = sb(st, "rbc", [128, 72], F32)
                t_Wr = T("Wr")
                stats = sb(st, "stats", [128, 4, 6], F32)
                mv = sb(st, "mv", [128, 2], F32)
                sc = sb(st, "sc", [128, 4], F32)
                t_stats, t_mv, t_sc = T("stats"), T("mv"), T("sc")
                t_sa, t_rs, t_sm, t_sm2, t_Pm, t_rsm, t_PmT = T(), T(), T(), T(), T(), T(), T()
                t_rt, t_rti, t_Ab, t_s3 = T(), T(), T(), T()
                Pm = sb(st, "Pm", [128, 4, 256], BF16)
                PmT = sb(st, "PmT", [128, 8, 128], BF16)
                sm = sb(st, "sm", [128, 16], F32)
                rt = sb(st, "rt", [128, 512], F32)
                rti = sb(st, "rti", [128, 8], I32)
                Ab = sb(st, "Ab", [128, NE], BF16)
                Acum = sb(st, "Acum", [128, NE], BF16)
                t_Acum = T("Acum")
                st2 = contextlib.ExitStack()
                memT = sb(st2, "memT", [128, KC, 256], BF16)
                t_memT = T("memT")

                if first_layer:
                    P.op("vector", lambda e: e.memset(r[:], 0.0), writes=[t_r])
                    P.dma("sync", [lambda e: e.dma_start(out=x1d[0:128, :], in_=r[:]),
                                   lambda e: e.dma_start(out=x1d[128:256, :], in_=r[:])],
                          reads=[t_r], writes=[t_x1[0], t_x1[1]], acc=[t_x1all])
                P.dma("sync", [lambda e: e.dma_start(out=lng[:], in_=W[L]["ln_g"][0].partition_broadcast(128)),
                               lambda e: e.dma_start(out=lnb[:], in_=W[L]["ln_b"][0].partition_broadcast(128))],
                      writes=[t_ln])
                P.dma("sync", [lambda e: e.dma_start(out=Wr[:, :, 0:8], in_=W[L]["w_rg"].rearrange("(kc p) n -> p kc n", p=128)),
                               lambda e: e.dma_start(out=Wr[:, :, 8:72], in_=W[L]["w_re"].rearrange("(kc p) n -> p kc n", p=128)),
                               lambda e: e.dma_start(out=rbc[:, 0:8], in_=W[L]["b_rg"].partition_broadcast(128)),
                               lambda e: e.dma_start(out=rbc[:, 8:72], in_=W[L]["b_re"].partition_broadcast(128))],
                      writes=[t_Wr])
                P.dma("sync", lambda e: e.dma_start(out=rowtab, in_=c_oob), writes=[t_rowtab])
                P.op("vector", lambda e: e.memset(Acum[:], 0.0), writes=[t_Acum])

                for mb in range(2):
                    P.dma("sync", lambda e, mb=mb: e.dma_start(out=xt[:], in_=mem_in[mb * 128:(mb + 1) * 128, :]),
                          writes=[t_xt])
                    for g4 in range(4):
                        pt, tp = psf.next()
                        for q4 in range(4):
                            kc = g4 * 4 + q4
                            P.op("tensor", lambda e, pt=pt, kc=kc, q4=q4: e.transpose(
                                out=pt[:, q4 * 128:(q4 + 1) * 128], in_=xt[:, kc * 128:(kc + 1) * 128], identity=ident[:]),
                                reads=[t_xt, t_const], writes=[tp] if q4 == 0 else (), acc=[tp] if q4 else ())
                        evac(memT[:, g4 * 4:(g4 + 1) * 4, mb * 128:(mb + 1) * 128],
                             pt[:].rearrange("p (a b) -> p a b", a=4), [tp], [])
                        P_last = None
                P.barrier()
                wk_i = ws.add(wchunk_dma(W[L]["w_mem_kv"][:, 0:512]))
                wv_i = ws.add(wchunk_dma(W[L]["w_mem_kv"][:, 512:1024]))
                wk, t_wk = ws.get(wk_i)
                for h in range(4):
                    pt, tp = psf.next()
                    for kc in range(KC):
                        P.op("tensor", lambda e, pt=pt, kc=kc, h=h: e.matmul(
                            pt[:, 0:256], lhsT=wk[:, kc, h * 128:(h + 1) * 128], rhs=memT[:, kc, :],
                            start=(kc == 0), stop=(kc == KC - 1)),
                            reads=[t_wk, t_memT], writes=[tp] if kc == 0 else (), acc=[tp] if kc else ())
                    evac(kmT[:, h, :], pt[:, 0:256], [tp], [T()])
                wv, t_wv = ws.get(wv_i)
                for mb in range(2):
                    pt, tp = psf.next()
                    for kc in range(KC):
                        P.op("tensor", lambda e, pt=pt, kc=kc, mb=mb: e.matmul(
                            pt[:, :], lhsT=memT[:, kc, mb * 128:(mb + 1) * 128], rhs=wv[:, kc, :],
                            start=(kc == 0), stop=(kc == KC - 1)),
                            reads=[t_wv, t_memT], writes=[tp] if kc == 0 else (), acc=[tp] if kc else ())
                    evac(vm[:, mb, :], pt[:, :], [tp], [T()])
                P.barrier()
                P.emit()
                st2.close()

                if is_attn:
                    qT = sb(st, "qT", [128, 12, 512], BF16)
                    t_qT = T("qT")
                    kT = sb(st, "kT", [128, 4, 640], BF16)
                    t_kT = T("kT")
                    vtok = sb(st, "vtok", [128, 5, 256], BF16)
                    t_vtok = T("vtok")
                    L8 = sb(st, "L8", [128, 6, 256], F32)
                    t_L8 = T("L8")
                    Pp = sb(st, "Pp", [128, 6, 256], BF16)
                    t_Pp = T("Pp")
                    PT = sb(st, "PT", [128, 12, 128], BF16)
                    t_PT = T("PT")
                    sa = sb(st, "sa", [128, 48], F32)
                    sink8 = sb(st, "sink8", [128, NQH], F32)
                    t_sink = T("sink")
                    P.dma("sync", lambda e: e.dma_start(out=sink8[:], in_=W[L]["sinks"].partition_broadcast(128)),
                          writes=[t_sink])
                    P.op("vector", lambda e: e.tensor_scalar(out=sink8[:], in0=sink8[:], scalar1=8.0, scalar2=None,
                                                             op0=ALU.mult), reads=[t_sink], writes=[t_sink])
                    P.op("vector", lambda e: e.memset(kT[:], 0.0), writes=[t_kT])
                    P.op("vector", lambda e: e.memset(vtok[:], 0.0), writes=[t_vtok])
                else:
                    uT = sb(st, "uT", [128, 12, 512], BF16)
                    t_uT = T("uT")
                    vg = sb(st, "vg", [128, TOK_W], F32)
                    t_vg = T("vg")
                    vln = sb(st, "vln", [128, TOK_W], BF16)
                    t_vln = T("vln")
                    bsbc = sb(st, "bsbc", [128, TOK_W], F32)
                    bng = sb(st, "bng", [128, TOK_W], F32)
                    bnb = sb(st, "bnb", [128, TOK_W], F32)
                    t_bn = T("bn")
                    WsT = sb(st, "WsT", [128, 12, 128], BF16)
                    t_WsT = T("WsT")
                    svt = sb(st, "svt", [128, 512], F32)
                    t_svt = T("svt")
                    stats3 = sb(st, "stats3", [128, 3, 6], F32)
                    P.dma("sync", [lambda e: e.dma_start(out=bsbc[:], in_=W[L]["b_sp"].partition_broadcast(128)),
                                   lambda e: e.dma_start(out=bng[:], in_=W[L]["norm_g"].partition_broadcast(128)),
                                   lambda e: e.dma_start(out=bnb[:], in_=W[L]["norm_b"].partition_broadcast(128))],
                          writes=[t_bn])
                    for g in range(12):
                        P.dma("sync", lambda e, g=g: e.dma_start(out=tmpc[:], in_=W[L]["w_sp"][g]), writes=[t_tmpc])
                        pt, tp = psf.next()
                        P.op("tensor", lambda e, pt=pt: e.transpose(out=pt[:, 0:128], in_=tmpc[:], identity=ident[:]),
                             reads=[t_tmpc, t_const], writes=[tp])
                        P.op("vector", lambda e, pt=pt, g=g: e.tensor_tensor(out=WsT[:, g, :], in0=pt[:, 0:128],
                                                                            in1=triu[:], op=ALU.mult),
                             reads=[tp, t_const], acc=[t_WsT])

                sblocks = [inblk[i:i + 4] for i in range(0, len(inblk), 4)]
                sched = []
                for sbk in sblocks:
                    ent = {}
                    if is_attn:
                        ent["in"] = [ws.add(wchunk_dma(W[L]["w_in"][:, c * 512:(c + 1) * 512])) for c in range(5)]
                    else:
                        ent["in"] = [ws.add(wchunk_dma(W[L]["w_in"][:, c * 512:(c + 1) * 512])) for c in range(7)]
                    if any(b in fullblk for b in sbk):
                        ent["out"] = [ws.add(wchunk_dma(W[L]["w_out"][:, c * 512:(c + 1) * 512])) for c in range(4)]
                    sched.append(ent)

                for si, sbk in enumerate(sblocks):
                    nb = len(sbk)
                    ntk = nb * 128
                    ent = sched[si]
                    for bi, blk in enumerate(sbk):
                        b = blk + 2
                        P.dma("sync", lambda e, b=b: e.dma_start(out=xt[:], in_=src_x[b * 128:(b + 1) * 128, :]),
                              reads=[t_src[b]], writes=[t_xt])
                        for g4 in range(4):
                            pt, tp = psf.next()
                            for q4 in range(4):
                                kc = g4 * 4 + q4
                                P.op("tensor", lambda e, pt=pt, kc=kc, q4=q4: e.transpose(
                                    out=pt[:, q4 * 128:(q4 + 1) * 128], in_=xt[:, kc * 128:(kc + 1) * 128],
                                    identity=ident[:]),
                                    reads=[t_xt], writes=[tp] if q4 == 0 else (), acc=[tp] if q4 else ())
                            evac(xT[:, g4 * 4:(g4 + 1) * 4, bi * 128:(bi + 1) * 128],
                                 pt[:].rearrange("p (a b) -> p a b", a=4), [tp], acc=[t_xT])

                    if dbg_stop == "X" and si == 0:
                        for kc in range(KC):
                            P.op("vector", lambda e, kc=kc: e.tensor_copy(out=r[:, 0:512], in_=xT[:, kc, :]), reads=[t_xT], writes=[t_r])
                            P.dma("sync", lambda e, kc=kc: e.dma_start(out=y_out[kc * 128:(kc + 1) * 128, 0:512], in_=r[:, 0:512]),
                                  reads=[t_r], acc=[t_yout])
                        P.barrier()
                        P.emit()
                        return nc
                    if is_attn:
                        for c in range(3):
                            wbuf, t_w = ws.get(ent["in"][c])
                            for cc in range(4):
                                pt, tp = psf.next()
                                for kc in range(KC):
                                    P.op("tensor", lambda e, pt=pt, kc=kc, cc=cc, wbuf=wbuf, ntk=ntk: e.matmul(
                                        pt[:, 0:ntk], lhsT=wbuf[:, kc, cc * 128:(cc + 1) * 128], rhs=xT[:, kc, 0:ntk],
                                        start=(kc == 0), stop=(kc == KC - 1)),
                                        reads=[t_w, t_xT], writes=[tp] if kc == 0 else (), acc=[tp] if kc else ())
                                evac(qT[:, c * 4 + cc, 0:ntk], pt[:, 0:ntk], [tp], acc=[t_qT])
                        wbuf, t_w = ws.get(ent["in"][3])
                        for g in range(4):
                            pt, tp = psf.next()
                            for half in range(2):
                                for kc in range(KC):
                                    first = (half == 0 and kc == 0)
                                    P.op("tensor", lambda e, pt=pt, kc=kc, g=g, half=half, wbuf=wbuf, ntk=ntk: e.matmul(
                                        pt[half * 64:(half + 1) * 64, 0:ntk], lhsT=wbuf[:, kc, g * 64:(g + 1) * 64],
                                        rhs=xT[:, kc, 0:ntk], start=(kc == 0), stop=(kc == KC - 1)),
                                        reads=[t_w, t_xT], writes=[tp] if first else (), acc=() if first else [tp])
                            evac(kT[:, g, 128:128 + ntk], pt[:, 0:ntk], [tp], acc=[t_kT])
                        for bi in range(nb):
                            pt, tp = psf.next()
                            for kc in range(KC):
                                P.op("tensor", lambda e, pt=pt, kc=kc, bi=bi, wbuf=wbuf: e.matmul(
                                    pt[:, 0:256], lhsT=xT[:, kc, bi * 128:(bi + 1) * 128], rhs=wbuf[:, kc, 256:512],
                                    start=(kc == 0), stop=(kc == KC - 1)),
                                    reads=[t_w, t_xT], writes=[tp] if kc == 0 else (), acc=[tp] if kc else ())
                            evac(vtok[:, bi + 1, :], pt[:, 0:256], [tp], acc=[t_vtok])
                        qm_chunk = ent["in"][4]
                    else:
                        for c in range(3):
                            wbuf, t_w = ws.get(ent["in"][c])
                            for cc in range(4):
                                pt, tp = psf.next()
                                for kc in range(KC):
                                    P.op("tensor", lambda e, pt=pt, kc=kc, cc=cc, wbuf=wbuf, ntk=ntk: e.matmul(
                                        pt[:, 0:ntk], lhsT=wbuf[:, kc, cc * 128:(cc + 1) * 128], rhs=xT[:, kc, 0:ntk],
                                        start=(kc == 0), stop=(kc == KC - 1)),
                                        reads=[t_w, t_xT], writes=[tp] if kc == 0 else (), acc=[tp] if kc else ())
                                P.op("scalar", lambda e, pt=pt, c=c, cc=cc, ntk=ntk: e.activation(
                                    out=uT[:, c * 4 + cc, 0:ntk], in_=pt[:, 0:ntk], func=AF.Gelu),
                                    reads=[tp], writes=[t_uT] if (c == 0 and cc == 0) else (),
                                    acc=() if (c == 0 and cc == 0) else [t_uT])
                        qm_chunk = ent["in"][6]

                    if not is_attn:
                        for bi, blk in enumerate(sbk):
                            if blk not in fullblk:
                                continue
                            for c in range(3):
                                wbuf, t_w = ws.get(ent["in"][3 + c])
                                pt, tp = psf.next()
                                for kc in range(KC):
                                    P.op("tensor", lambda e, pt=pt, kc=kc, bi=bi, wbuf=wbuf: e.matmul(
                                        pt[:, :], lhsT=xT[:, kc, bi * 128:(bi + 1) * 128], rhs=wbuf[:, kc, :],
                                        start=(kc == 0), stop=(kc == KC - 1)),
                                        reads=[t_w, t_xT], writes=[tp] if kc == 0 else (), acc=[tp] if kc else ())
                                P.op("scalar", lambda e, pt=pt, c=c: e.activation(
                                    out=vg[:, c * 512:(c + 1) * 512], in_=pt[:, :], func=AF.Gelu),
                                    reads=[tp], writes=[t_vg] if c == 0 else (), acc=[t_vg] if c else ())
                            for c in range(3):
                                P.op("vector", lambda e, c=c: e.bn_stats(out=stats3[:, c, :], in_=vg[:, c * 512:(c + 1) * 512]),
                                     reads=[t_vg], acc=[t_s3])
                            P.op("vector", lambda e: e.bn_aggr(out=mv[:, 0:2], in_=stats3[:].rearrange("p a b -> p (a b)")),
                                 reads=[t_s3], writes=[t_mv])
                            P.op("vector", lambda e: e.tensor_scalar(out=sc[:, 0:1], in0=mv[:, 1:2], scalar1=LN_EPS,
                                                                     scalar2=None, op0=ALU.add), reads=[t_mv], writes=[t_sc])
                            P.op("scalar", lambda e: e.activation(out=sc[:, 1:2], in_=sc[:, 0:1], func=AF.Ln),
                                 reads=[t_sc], writes=[t_sc])
                            P.op("scalar", lambda e: e.activation(out=sc[:, 2:3], in_=sc[:, 1:2], func=AF.Exp, scale=-0.5),
                                 reads=[t_sc], writes=[t_sc])
                            P.op("vector", lambda e: e.scalar_tensor_tensor(out=sc[:, 3:4], in0=mv[:, 0:1], scalar=-1.0,
                                                                            in1=sc[:, 2:3], op0=ALU.mult, op1=ALU.mult),
                                 reads=[t_mv, t_sc], writes=[t_sc])
                            P.op("scalar", lambda e: e.activation(out=vg[:], in_=vg[:], func=AF.Identity,
                                                                  bias=sc[:, 3:4], scale=sc[:, 2:3]),
                                 reads=[t_sc, t_vg], writes=[t_vg])
                            P.op("vector", lambda e: e.tensor_tensor(out=vg[:], in0=vg[:], in1=bng[:], op=ALU.mult),
                                 reads=[t_vg, t_bn], writes=[t_vg])
                            P.op("vector", lambda e: e.tensor_tensor(out=vln[:], in0=vg[:], in1=bnb[:], op=ALU.add),
                                 reads=[t_vg, t_bn], writes=[t_vln])
                            for g3 in range(3):
                                pt, tp = psf.next()
                                for gi in range(4):
                                    g = g3 * 4 + gi
                                    P.op("tensor", lambda e, pt=pt, g=g, gi=gi: e.matmul(
                                        pt[:, gi * 128:(gi + 1) * 128], lhsT=vln[:, g * 128:(g + 1) * 128],
                                        rhs=WsT[:, g, :], start=True, stop=True),
                                        reads=[t_vln, t_WsT], writes=[tp] if gi == 0 else (), acc=[tp] if gi else ())
                                P.op("vector", lambda e, pt=pt, g3=g3: e.tensor_tensor(
                                    out=svt[:], in0=pt[:], in1=bsbc[:, g3 * 512:(g3 + 1) * 512], op=ALU.add),
                                    reads=[tp, t_bn], writes=[t_svt])
                                P.op("vector", lambda e, g3=g3, bi=bi: e.tensor_tensor(
                                    out=mixT[:, g3 * 4:(g3 + 1) * 4, bi * 128:(bi + 1) * 128],
                                    in0=svt[:].rearrange("p (a b) -> p a b", a=4),
                                    in1=uT[:, g3 * 4:(g3 + 1) * 4, bi * 128:(bi + 1) * 128], op=ALU.mult),
                                    reads=[t_svt, t_uT], writes=[t_mixT] if (g3 == 0 and bi == 0) else (),
                                    acc=() if (g3 == 0 and bi == 0) else [t_mixT])

                    wbuf, t_w = ws.get(qm_chunk)
                    for cc in range(4):
                        pt, tp = psf.next()
                        for kc in range(KC):
                            P.op("tensor", lambda e, pt=pt, kc=kc, cc=cc, wbuf=wbuf, ntk=ntk: e.matmul(
                                pt[:, 0:ntk], lhsT=wbuf[:, kc, cc * 128:(cc + 1) * 128], rhs=xT[:, kc, 0:ntk],
                                start=(kc == 0), stop=(kc == KC - 1)),
                                reads=[t_w, t_xT], writes=[tp] if kc == 0 else (), acc=[tp] if kc else ())
                        evac(qmT[:, cc, 0:ntk], pt[:, 0:ntk], [tp], acc=[t_qmT])

                    wout = None
                    for bi, blk in enumerate(sbk):
                        if blk not in fullblk:
                            continue
                        b = blk + 2
                        c0 = bi * 128
                        if is_attn:
                            dtile = dist1 if blk == 0 else dist
                            for g in range(4):
                                for hh in range(6):
                                    h = g * 6 + hh
                                    cq = h // 2
                                    hf = h % 2
                                    pt, tp = psf.next()
                                    P.op("tensor", lambda e, pt=pt, cq=cq, hf=hf, g=g, c0=c0: e.matmul(
                                        pt[:, 0:256],
                                        lhsT=qT[hf * 64:(hf + 1) * 64, cq, c0:c0 + 128],
                                        rhs=kT[hf * 64:(hf + 1) * 64, g, c0:c0 + 256], start=True, stop=True),
                                        reads=[t_qT, t_kT], writes=[tp])
                                    first = (hh == 0)
                                    P.op("vector", lambda e, pt=pt, hh=hh, h=h, dtile=dtile: e.scalar_tensor_tensor(
                                        out=L8[:, hh, :], in0=dtile[:], scalar=-8.0 * SLOPES[h],
                                        in1=pt[:, 0:256], op0=ALU.mult, op1=ALU.add),
                                        reads=[tp, t_const], writes=[t_L8] if first else (), acc=() if first else [t_L8])
                                gs = slice(g * 6, (g + 1) * 6)
                                P.op("vector", lambda e: e.tensor_reduce(out=sa[:, 0:6], in_=L8[:], axis=AX.X, op=ALU.max),
                                     reads=[t_L8], writes=[t_sa])
                                P.op("vector", lambda e, gs=gs: e.tensor_tensor(out=sa[:, 0:6], in0=sa[:, 0:6], in1=sink8[:, gs],
                                                                               op=ALU.max), reads=[t_sa, t_sink], writes=[t_sa])
                                P.op("vector", lambda e: e.tensor_scalar(out=sa[:, 6:12], in0=sa[:, 0:6], scalar1=-0.125,
                                                                         scalar2=None, op0=ALU.mult), reads=[t_sa], writes=[t_sa])
                                P.op("vector", lambda e, gs=gs: e.tensor_tensor(out=sa[:, 12:18], in0=sink8[:, gs], in1=sa[:, 0:6],
                                                                               op=ALU.subtract), reads=[t_sa, t_sink], writes=[t_sa])
                                for hh in range(6):
                                    P.op("scalar", lambda e, hh=hh: e.activation(
                                        out=Pp[:, hh, :], in_=L8[:, hh, :], func=AF.Exp, bias=sa[:, 6 + hh:7 + hh], scale=0.125,
                                        accum_out=sa[:, 24 + hh:25 + hh]),
                                        reads=[t_L8, t_sa], writes=[t_Pp] if hh == 0 else (), acc=[t_rs] + ([t_Pp] if hh else []))
                                P.op("scalar", lambda e: e.activation(out=sa[:, 18:24], in_=sa[:, 12:18], func=AF.Exp, scale=0.125),
                                     reads=[t_sa], writes=[t_sa])
                                P.op("vector", lambda e: e.tensor_tensor(out=sa[:, 30:36], in0=sa[:, 24:30], in1=sa[:, 18:24],
                                                                         op=ALU.add), reads=[t_sa, t_rs], writes=[t_sa])
                                P.op("vector", lambda e: e.reciprocal(out=sa[:, 36:42], in_=sa[:, 30:36]), reads=[t_sa], writes=[t_sa])
                                for half in range(2):
                                    pb, tpb = psb.next()
                                    n = 8 if half == 0 else 4
                                    for i8 in range(n):
                                        idx = half * 8 + i8
                                        hh, kb = idx // 2, idx % 2
                                        P.op("tensor", lambda e, pb=pb, i8=i8, hh=hh, kb=kb: e.transpose(
                                            out=pb[:, i8 * 128:(i8 + 1) * 128], in_=Pp[:, hh, kb * 128:(kb + 1) * 128],
                                            identity=identb[:]),
                                            reads=[t_Pp], writes=[tpb] if i8 == 0 else (), acc=[tpb] if i8 else ())
                                    evac(PT[:, half * 8:half * 8 + n, :], pb[:, 0:n * 128].rearrange("p (a b) -> p a b", a=n),
                                         [tpb], acc=[t_PT])
                                pt, tp = psf.next()
                                for hh in range(6):
                                    for kb in range(2):
                                        first = (hh == 0 and kb == 0)
                                        P.op("tensor", lambda e, pt=pt, hh=hh, kb=kb, g=g, bi=bi: e.matmul(
                                            pt[:, hh * 64:(hh + 1) * 64], lhsT=PT[:, hh * 2 + kb, :],
                                            rhs=vtok[:, bi + kb, g * 64:(g + 1) * 64], start=(kb == 0), stop=(kb == 1)),
                                            reads=[t_PT, t_vtok], writes=[tp] if first else (), acc=() if first else [tp])
                                P.op("vector", lambda e, pt=pt, g=g: e.tensor_tensor(
                                    out=mix[:, g * 384:(g + 1) * 384].rearrange("p (a b) -> p a b", a=6),
                                    in0=pt[:, 0:384].rearrange("p (a b) -> p a b", a=6),
                                    in1=sa[:, 36:42].unsqueeze(2).to_broadcast([128, 6, 64]), op=ALU.mult),
                                    reads=[tp, t_sa], writes=[t_mix] if g == 0 else (), acc=[t_mix] if g else ())

                        SCM = 128.0 ** -0.5
                        pms = []
                        for hp in range(2):
                            pt, tp = psf.next()
                            pms.append((pt, tp))
                            for h2 in range(2):
                                h = hp * 2 + h2
                                P.op("tensor", lambda e, pt=pt, h=h, h2=h2, c0=c0: e.matmul(
                                    pt[:, h2 * 256:(h2 + 1) * 256], lhsT=qmT[:, h, c0:c0 + 128], rhs=kmT[:, h, :],
                                    start=True, stop=True),
                                    reads=[t_qmT, t_km], writes=[tp] if h2 == 0 else (), acc=[tp] if h2 else ())
                            P.op("vector", lambda e, pt=pt, hp=hp: e.tensor_reduce(
                                out=sm[:, hp * 2:hp * 2 + 2], in_=pt[:].rearrange("p (a b) -> p a b", a=2), axis=AX.X, op=ALU.max),
                                reads=[tp], acc=[t_sm])
                        P.op("vector", lambda e: e.tensor_scalar(out=sm[:, 4:8], in0=sm[:, 0:4], scalar1=-SCM, scalar2=None,
                                                                 op0=ALU.mult), reads=[t_sm], writes=[t_sm2])
                        for h in range(4):
                            pt, tp = pms[h // 2]
                            P.op("scalar", lambda e, pt=pt, h=h: e.activation(
                                out=Pm[:, h, :], in_=pt[:, (h % 2) * 256:(h % 2 + 1) * 256], func=AF.Exp,
                                bias=sm[:, 4 + h:5 + h], scale=SCM, accum_out=sm[:, 8 + h:9 + h]),
                                reads=[tp, t_sm2], acc=[t_Pm, t_rsm])
                        P.op("vector", lambda e: e.reciprocal(out=sm[:, 12:16], in_=sm[:, 8:12]), reads=[t_rsm], writes=[t_sm2])
                        pb, tpb = psb.next()
                        for i8 in range(8):
                            h, mb = i8 // 2, i8 % 2
                            P.op("tensor", lambda e, pb=pb, i8=i8, h=h, mb=mb: e.transpose(
                                out=pb[:, i8 * 128:(i8 + 1) * 128], in_=Pm[:, h, mb * 128:(mb + 1) * 128], identity=identb[:]),
                                reads=[t_Pm], writes=[tpb] if i8 == 0 else (), acc=[tpb] if i8 else ())
                        evac(PmT[:, :, :], pb[:].rearrange("p (a b) -> p a b", a=8), [tpb], [t_PmT])
                        pt, tp = psf.next()
                        for h in range(4):
                            for mb in range(2):
                                first = (h == 0 and mb == 0)
                                P.op("tensor", lambda e, pt=pt, h=h, mb=mb: e.matmul(
                                    pt[:, h * 128:(h + 1) * 128], lhsT=PmT[:, h * 2 + mb, :], rhs=vm[:, mb, h * 128:(h + 1) * 128],
                                    start=(mb == 0), stop=(mb == 1)),
                                    reads=[t_PmT, t_km], writes=[tp] if first else (), acc=() if first else [tp])
                        P.op("vector", lambda e, pt=pt: e.tensor_tensor(
                            out=mix[:, 1536 - mo:2048 - mo].rearrange("p (a b) -> p a b", a=4),
                            in0=pt[:].rearrange("p (a b) -> p a b", a=4),
                            in1=sm[:, 12:16].unsqueeze(2).to_broadcast([128, 4, 128]), op=ALU.mult),
                            reads=[tp, t_sm2], writes=() if is_attn else [t_mix], acc=[t_mix] if is_attn else ())

                        if dbg_stop == "C":
                            P.op("vector", lambda e: e.tensor_copy(out=r[:], in_=mix[:]), reads=[t_mix], writes=[t_r])
                            if blk >= 0:
                                P.dma("sync", lambda e, blk=blk: e.dma_start(out=y_out[blk * 128:(blk + 1) * 128, :], in_=r[:]),
                                      reads=[t_r], acc=[t_yout])
                            continue
                        kcs = list(range(16)) if is_attn else list(range(12, 16))
                        mcol = 0 if is_attn else c0
                        for h8 in range(0, len(kcs), 8):
                            grp = kcs[h8:h8 + 8]
                            pb, tpb = psb.next()
                            for i8, kc in enumerate(grp):
                                P.op("tensor", lambda e, pb=pb, i8=i8, kc=kc: e.transpose(
                                    out=pb[:, i8 * 128:(i8 + 1) * 128], in_=mix[:, kc * 128 - mo:(kc + 1) * 128 - mo], identity=identb[:]),
                                    reads=[t_mix], writes=[tpb] if i8 == 0 else (), acc=[tpb] if i8 else ())
                            n = len(grp)
                            wr_first = is_attn and h8 == 0
                            evac(mixT[:, grp[0]:grp[0] + n, mcol:mcol + 128],
                                 pb[:, 0:n * 128].rearrange("p (a b) -> p a b", a=n), [tpb], acc=[t_mixT])

                        if wout is None:
                            wout = [ws.get(k, 0) for k in ent["out"]]
                        P.dma("sync", lambda e, b=b: e.dma_start(out=xt[:], in_=src_x[b * 128:(b + 1) * 128, :]),
                              reads=[t_src[b]], writes=[t_xt])
                        for c in range(4):
                            wbuf, t_w = wout[c]
                            pt, tp = psf.next()
                            for kc in range(KC):
                                P.op("tensor", lambda e, pt=pt, kc=kc, wbuf=wbuf, mcol=mcol: e.matmul(
                                    pt[:, :], lhsT=mixT[:, kc, mcol:mcol + 128], rhs=wbuf[:, kc, :],
                                    start=(kc == 0), stop=(kc == KC - 1)),
                                    reads=[t_w, t_mixT], writes=[tp] if kc == 0 else (), acc=[tp] if kc else ())
                            P.op("vector", lambda e, pt=pt, c=c: e.scalar_tensor_tensor(
                                out=r[:, c * 512:(c + 1) * 512], in0=xt[:, c * 512:(c + 1) * 512], scalar=ALPHA,
                                in1=pt[:, :], op0=ALU.mult, op1=ALU.add),
                                reads=[tp, t_xt], writes=[t_r] if c == 0 else (), acc=[t_r] if c else ())
                        layer_norm_rows((stats, mv, sc, t_stats, t_mv, t_sc), r, t_r, lng, lnb, t_ln, r[:], t_r, 0)
                        P.dma("sync", lambda e, b=b: e.dma_start(out=x1d[b * 128:(b + 1) * 128, :], in_=r[:]),
                              reads=[t_r], writes=[t_x1[b]], acc=[t_x1all])

                        for g4 in range(4):
                            pt, tp = psf.next()
                            for q4 in range(4):
                                kc = g4 * 4 + q4
                                P.op("tensor", lambda e, pt=pt, kc=kc, q4=q4: e.transpose(
                                    out=pt[:, q4 * 128:(q4 + 1) * 128], in_=r[:, kc * 128:(kc + 1) * 128], identity=ident[:]),
                                    reads=[t_r], writes=[tp] if q4 == 0 else (), acc=[tp] if q4 else ())
                            evac(x1T[:, g4 * 4:(g4 + 1) * 4, :], pt[:].rearrange("p (a b) -> p a b", a=4), [tp],
                                 acc=[t_x1T])
                        pt, tp = psf.next()
                        for kc in range(KC):
                            P.op("tensor", lambda e, pt=pt, kc=kc: e.matmul(
                                pt[:, 0:72], lhsT=x1T[:, kc, :], rhs=Wr[:, kc, :], start=(kc == 0), stop=(kc == KC - 1)),
                                reads=[t_x1T, t_Wr], writes=[tp] if kc == 0 else (), acc=[tp] if kc else ())
                        V = "vector"
                        S0 = 400
                        P.op(V, lambda e, pt=pt: e.tensor_tensor(out=rt[:, 0:72], in0=pt[:, 0:72], in1=rbc[:], op=ALU.add),
                             reads=[tp, t_Wr], writes=[t_rt])
                        P.op(V, lambda e: e.tensor_reduce(out=rt[:, S0:S0 + 1], in_=rt[:, 0:8], axis=AX.X, op=ALU.max),
                             reads=[t_rt], writes=[t_rt])
                        P.op(V, lambda e: e.tensor_scalar(out=rt[:, 72:80], in0=rt[:, 0:8], scalar1=rt[:, S0:S0 + 1], scalar2=None,
                                                          op0=ALU.is_ge), reads=[t_rt], writes=[t_rt])
                        P.op(V, lambda e: e.tensor_scalar(out=rt[:, S0 + 1:S0 + 2], in0=rt[:, S0:S0 + 1], scalar1=-1.0, scalar2=None,
                                                          op0=ALU.mult), reads=[t_rt], writes=[t_rt])
                        P.op("scalar", lambda e: e.activation(out=rt[:, 336:344], in_=rt[:, 0:8], func=AF.Exp,
                                                              bias=rt[:, S0 + 1:S0 + 2], scale=1.0,
                                                              accum_out=rt[:, S0 + 2:S0 + 3]), reads=[t_rt], writes=[t_rt])
                        P.op(V, lambda e: e.reciprocal(out=rt[:, S0 + 3:S0 + 4], in_=rt[:, S0 + 2:S0 + 3]), reads=[t_rt], writes=[t_rt])
                        P.op(V, lambda e: e.tensor_scalar(out=rt[:, 72:80], in0=rt[:, 72:80], scalar1=BIG, scalar2=-BIG,
                                                          op0=ALU.mult, op1=ALU.add), reads=[t_rt], writes=[t_rt])
                        P.op(V, lambda e: e.tensor_tensor(out=rt[:, 80:144].rearrange("p (a b) -> p a b", a=8),
                                                          in0=rt[:, 8:72].rearrange("p (a b) -> p a b", a=8),
                                                          in1=rt[:, 72:80].unsqueeze(2).to_broadcast([128, 8, 8]), op=ALU.add),
                             reads=[t_rt], writes=[t_rt])
                        P.op(V, lambda e: e.tensor_reduce(out=rt[:, S0 + 4:S0 + 5], in_=rt[:, 80:144], axis=AX.X, op=ALU.max),
                             reads=[t_rt], writes=[t_rt])
                        P.op(V, lambda e: e.tensor_scalar(out=rt[:, 144:208], in0=rt[:, 80:144], scalar1=rt[:, S0 + 4:S0 + 5],
                                                          scalar2=None, op0=ALU.is_ge), reads=[t_rt], writes=[t_rt])
                        P.op(V, lambda e: e.scalar_tensor_tensor(out=rt[:, 208:272], in0=rt[:, 144:208], scalar=-BIG,
                                                                 in1=rt[:, 80:144], op0=ALU.mult, op1=ALU.add),
                             reads=[t_rt], writes=[t_rt])
                        P.op(V, lambda e: e.tensor_reduce(out=rt[:, S0 + 5:S0 + 6], in_=rt[:, 208:272], axis=AX.X, op=ALU.max),
                             reads=[t_rt], writes=[t_rt])
                        P.op(V, lambda e: e.tensor_scalar(out=rt[:, 272:336], in0=rt[:, 208:272], scalar1=rt[:, S0 + 5:S0 + 6],
                                                          scalar2=None, op0=ALU.is_ge), reads=[t_rt], writes=[t_rt])
                        P.op(V, lambda e: e.tensor_tensor(out=rt[:, S0 + 6:S0 + 7], in0=rt[:, S0 + 5:S0 + 6], in1=rt[:, S0 + 4:S0 + 5],
                                                          op=ALU.subtract), reads=[t_rt], writes=[t_rt])
                        P.op("scalar", lambda e: e.activation(out=rt[:, S0 + 7:S0 + 8], in_=rt[:, S0 + 6:S0 + 7], func=AF.Exp),
                             reads=[t_rt], writes=[t_rt])
                        P.op(V, lambda e: e.tensor_scalar(out=rt[:, S0 + 8:S0 + 9], in0=rt[:, S0 + 7:S0 + 8], scalar1=1.0, scalar2=None,
                                                          op0=ALU.add), reads=[t_rt], writes=[t_rt])
                        P.op(V, lambda e: e.reciprocal(out=rt[:, S0 + 9:S0 + 10], in_=rt[:, S0 + 8:S0 + 9]), reads=[t_rt], writes=[t_rt])
                        P.op(V, lambda e, b=b: e.tensor_tensor(out=gates[:, 2 * b:2 * b + 1], in0=rt[:, S0 + 9:S0 + 10],
                                                               in1=rt[:, S0 + 3:S0 + 4], op=ALU.mult),
                             reads=[t_rt], writes=[t_gates[b]])
                        P.op(V, lambda e, b=b: e.tensor_tensor(out=gates[:, 2 * b + 1:2 * b + 2], in0=gates[:, 2 * b:2 * b + 1],
                                                               in1=rt[:, S0 + 7:S0 + 8], op=ALU.mult),
                             reads=[t_rt, t_gates[b]], writes=[t_gates[b]])
                        P.op(V, lambda e: e.tensor_tensor(out=Ab[:], in0=rt[:, 144:208], in1=rt[:, 272:336], op=ALU.add),
                             reads=[t_rt], writes=[t_Ab])
                        pt, tp = psf.next()
                        P.op("tensor", lambda e, pt=pt: e.matmul(pt[:, 0:NE], lhsT=lstrict[:], rhs=Ab[:], start=True, stop=False),
                             reads=[t_Ab], writes=[tp])
                        P.op("tensor", lambda e, pt=pt: e.matmul(pt[:, 0:NE], lhsT=onesb[:], rhs=Acum[:], start=False, stop=True),
                             reads=[t_Acum], acc=[tp])
                        for k in range(2):
                            mk = rt[:, 144:208] if k == 0 else rt[:, 272:336]
                            c_pos = S0 + 10 + 4 * k
                            P.op(V, lambda e, pt=pt, mk=mk: e.tensor_tensor(out=rt[:, 336:400], in0=pt[:, 0:NE], in1=mk, op=ALU.mult),
                                 reads=[tp, t_rt], writes=[t_rt])
                            P.op(V, lambda e, c_pos=c_pos: e.tensor_reduce(out=rt[:, c_pos:c_pos + 1], in_=rt[:, 336:400], axis=AX.X,
                                                                           op=ALU.add), reads=[t_rt], writes=[t_rt])
                            P.op(V, lambda e, mk=mk: e.tensor_tensor(out=rt[:, 336:400], in0=iota[:], in1=mk, op=ALU.mult),
                                 reads=[t_rt, t_const], writes=[t_rt])
                            P.op(V, lambda e, c_pos=c_pos: e.tensor_reduce(out=rt[:, c_pos + 1:c_pos + 2], in_=rt[:, 336:400], axis=AX.X,
                                                                           op=ALU.add), reads=[t_rt], writes=[t_rt])
                            P.op(V, lambda e, c_pos=c_pos: e.tensor_scalar(out=rt[:, c_pos + 2:c_pos + 3], in0=rt[:, c_pos:c_pos + 1],
                                                                           scalar1=float(cap), scalar2=None, op0=ALU.is_lt),
                                 reads=[t_rt], writes=[t_rt])
                            P.op(V, lambda e, c_pos=c_pos: e.scalar_tensor_tensor(out=rt[:, c_pos + 3:c_pos + 4], in0=rt[:, c_pos + 1:c_pos + 2],
                                                                                  scalar=float(cap), in1=rt[:, c_pos:c_pos + 1],
                                                                                  op0=ALU.mult, op1=ALU.add), reads=[t_rt], writes=[t_rt])
                            P.op(V, lambda e, c_pos=c_pos, k=k: e.tensor_tensor(out=rt[:, c_pos + 3:c_pos + 4], in0=rt[:, c_pos + 3:c_pos + 4],
                                                                                in1=pidx[:, k:k + 1], op=ALU.subtract),
                                 reads=[t_rt, t_const], writes=[t_rt])
                            P.op(V, lambda e, c_pos=c_pos, k=k: e.scalar_tensor_tensor(out=rt[:, c_pos + 3:c_pos + 4], in0=rt[:, c_pos + 3:c_pos + 4],
                                                                                       scalar=rt[:, c_pos + 2:c_pos + 3], in1=pidx[:, k:k + 1],
                                                                                       op0=ALU.mult, op1=ALU.add),
                                 reads=[t_rt, t_const], writes=[t_rt])
                            P.op(V, lambda e, c_pos=c_pos, k=k: e.tensor_copy(out=rti[:, k:k + 1], in_=rt[:, c_pos + 3:c_pos + 4]),
                                 reads=[t_rt], writes=[t_rti])
                            P.dma("gpsimd", lambda e, k=k, b=b: e.indirect_dma_start(
                                out=rowtab, out_offset=bass.IndirectOffsetOnAxis(ap=rti[:, k:k + 1], axis=0),
                                in_=tokidx[:, b * 4 + 2 * k:b * 4 + 2 * k + 2], in_offset=None,
                                bounds_check=None),
                                reads=[t_rti, t_const], acc=[t_rowtab])
                        P.op(V, lambda e: e.tensor_tensor(out=Acum[:], in0=Acum[:], in1=Ab[:], op=ALU.add),
                             reads=[t_Ab, t_Acum], writes=[t_Acum])

                    if is_attn and si + 1 < len(sblocks):
                        P.op("vector", lambda e, nb=nb: e.tensor_copy(out=kT[:, :, 0:128], in_=kT[:, :, nb * 128:(nb + 1) * 128]),
                             reads=[t_kT], writes=[t_kT])
                        P.op("vector", lambda e, nb=nb: e.tensor_copy(out=vtok[:, 0, :], in_=vtok[:, nb, :]),
                             reads=[t_vtok], writes=[t_vtok])
                P.barrier()
                P.emit()

            if dbg_stop == "C":
                return nc
            if dbg_stop == "M":
                P.dma("sync", lambda e: e.dma_start(out=y_out, in_=x1d[256:NTOK, :]), reads=[t_x1all], acc=[t_yout])
                P.barrier()
                P.emit()
                return nc
            nrb = cap // 128
            with contextlib.ExitStack() as st:
                wb = [(sb(st, "ewb%d" % i, [128, KC, 512], BF16), T("ewb%d" % i)) for i in range(6)]
                ws = WStream(P, wb, depth=3)
                for ex in range(NE):
                    ws.add(wchunk_dma(W[L]["w_gate"][ex]))
                    ws.add(wchunk_dma(W[L]["w_up"][ex]))
                    ws.add(wdown_dma(W[L]["w_down"][ex]))
                idxr = Ring([(sb(st, "idx%d" % i, [128, 2], I32), T()) for i in range(2 * nrb)])
                xgr = Ring([(sb(st, "xg%d" % i, [128, D], BF16), T()) for i in range(3)])
                xrTr = Ring([(sb(st, "xrT%d" % i, [128, KC, cap], BF16), T()) for i in range(2)])
                hTr = Ring([(sb(st, "hT%d" % i, [128, 4, cap], BF16), T()) for i in range(2)])
                sgr = Ring([(sb(st, "sg%d" % i, [128, cap], F32), T()) for i in range(2)])
                ysr = Ring([(sb(st, "ys%d" % i, [128, D], F32), T()) for i in range(3)])
                for xg, t_xg in xgr.items:
                    P.op("vector", lambda e, xg=xg: e.memset(xg[:], 0.0), writes=[t_xg])
                for ex in range(NE):
                    idxs = []
                    xrT, t_xrT = xrTr.next()
                    for rb in range(nrb):
                        idx, t_idx = idxr.next()
                        idxs.append((idx, t_idx))
                        row0 = ex * cap + rb * 128
                        P.dma("sync", lambda e, idx=idx, row0=row0: e.dma_start(out=idx[:], in_=rowtab[row0:row0 + 128, :]),
                              reads=[t_rowtab], writes=[t_idx])
                        xg, t_xg = xgr.next()
                        P.dma("gpsimd", lambda e, idx=idx, xg=xg: e.indirect_dma_start(
                            out=xg[:], out_offset=None, in_=x1d,
                            in_offset=bass.IndirectOffsetOnAxis(ap=idx[:, 0:1], axis=0),
                            bounds_check=None),
                            reads=[t_idx, t_x1all], writes=[t_xg])
                        for h8 in range(2):
                            pb, tpb = psb.next()
                            for i8 in range(8):
                                kc = h8 * 8 + i8
                                P.op("tensor", lambda e, pb=pb, i8=i8, kc=kc, xg=xg: e.transpose(
                                    out=pb[:, i8 * 128:(i8 + 1) * 128], in_=xg[:, kc * 128:(kc + 1) * 128], identity=identb[:]),
                                    reads=[t_xg], writes=[tpb] if i8 == 0 else (), acc=[tpb] if i8 else ())
                            first = (rb == 0 and h8 == 0)
                            evac(xrT[:, h8 * 8:(h8 + 1) * 8, rb * 128:(rb + 1) * 128],
                                 pb[:].rearrange("p (a b) -> p a b", a=8), [tpb], acc=[t_xrT])
                    wg, t_wg = ws.get(3 * ex)
                    wu, t_wu = ws.get(3 * ex + 1)
                    hT, t_hT = hTr.next()
                    for fc in range(4):
                        pg, tpg = psf.next()
                        pu, tpu = psf.next()
                        for kc in range(KC):
                            P.op("tensor", lambda e, pg=pg, kc=kc, fc=fc, wg=wg, xrT=xrT: e.matmul(
                                pg[:, 0:cap], lhsT=wg[:, kc, fc * 128:(fc + 1) * 128], rhs=xrT[:, kc, :],
                                start=(kc == 0), stop=(kc == KC - 1)),
                                reads=[t_wg, t_xrT], writes=[tpg] if kc == 0 else (), acc=[tpg] if kc else ())
                        for kc in range(KC):
                            P.op("tensor", lambda e, pu=pu, kc=kc, fc=fc, wu=wu, xrT=xrT: e.matmul(
                                pu[:, 0:cap], lhsT=wu[:, kc, fc * 128:(fc + 1) * 128], rhs=xrT[:, kc, :],
                                start=(kc == 0), stop=(kc == KC - 1)),
                                reads=[t_wu, t_xrT], writes=[tpu] if kc == 0 else (), acc=[tpu] if kc else ())
                        sg, t_sg = sgr.next()
                        P.op("scalar", lambda e, pg=pg, sg=sg: e.activation(out=sg[:, :], in_=pg[:, 0:cap], func=AF.Silu),
                             reads=[tpg], writes=[t_sg])
                        P.op("vector", lambda e, pu=pu, sg=sg, hT=hT, fc=fc: e.tensor_tensor(
                            out=hT[:, fc, :], in0=sg[:, :], in1=pu[:, 0:cap], op=ALU.mult),
                            reads=[tpu, t_sg], writes=[t_hT] if fc == 0 else (), acc=[t_hT] if fc else ())
                    wd, t_wd = ws.get(3 * ex + 2)
                    wdv = wd[:].rearrange("p a b -> p (a b)").rearrange("p (fc n) -> p fc n", fc=4)
                    for rb in range(nrb):
                        ys, t_ys = ysr.next()
                        for c in range(4):
                            pt, tp = psf.next()
                            for fc in range(4):
                                P.op("tensor", lambda e, pt=pt, fc=fc, c=c, rb=rb, hT=hT, wdv=wdv: e.matmul(
                                    pt[:, :], lhsT=hT[:, fc, rb * 128:(rb + 1) * 128], rhs=wdv[:, fc, c * 512:(c + 1) * 512],
                                    start=(fc == 0), stop=(fc == 3)),
                                    reads=[t_wd, t_hT], writes=[tp] if fc == 0 else (), acc=[tp] if fc else ())
                            evac(ys[:, c * 512:(c + 1) * 512], pt[:, :], [tp], acc=[t_ys])
                        idx, t_idx = idxs[rb]
                        P.dma("gpsimd", lambda e, idx=idx, ys=ys: e.indirect_dma_start(
                            out=ybuf, out_offset=bass.IndirectOffsetOnAxis(ap=idx[:, 1:2], axis=0),
                            in_=ys[:], in_offset=None, bounds_check=None),
                            reads=[t_idx, t_ys], writes=[t_trash], acc=[t_ybuf])
                P.barrier()
                P.emit()

            with contextlib.ExitStack() as st:
                lng = sb(st, "lng2", [128, D], F32)
                lnb = sb(st, "lnb2", [128, D], F32)
                t_ln = T()
                P.dma("sync", [lambda e: e.dma_start(out=lng[:], in_=W[L]["ln_g"][1].partition_broadcast(128)),
                               lambda e: e.dma_start(out=lnb[:], in_=W[L]["ln_b"][1].partition_broadcast(128))],
                      writes=[t_ln])
                x1r = Ring([(sb(st, "x1b%d" % i, [128, D], F32), T()) for i in range(2)])
                ybr = Ring([(sb(st, "yb%d" % i, [128, 2 * D], F32), T()) for i in range(2)])
                rr = Ring([(sb(st, "r2_%d" % i, [128, D], F32), T()) for i in range(2)])
                stats = sb(st, "stats2", [128, 4, 6], F32)
                mv = sb(st, "mv2", [128, 2], F32)
                sc = sb(st, "sc2", [128, 4], F32)
                t_stats, t_mv, t_sc = T("stats"), T("mv"), T("sc")
                for blk in fullblk:
                    b = blk + 2
                    x1b, t_x1b = x1r.next()
                    yb, t_yb = ybr.next()
                    r2, t_r2 = rr.next()
                    P.dma("sync", lambda e, b=b, x1b=x1b: e.dma_start(out=x1b[:], in_=x1d[b * 128:(b + 1) * 128, :]),
                          reads=[t_x1[b]], writes=[t_x1b])
                    P.dma("sync", lambda e, b=b, yb=yb: e.dma_start(
                        out=yb[:], in_=ybuf[b * 256:(b + 1) * 256, :].rearrange("(p k) n -> p (k n)", k=2)),
                        reads=[t_ybuf], writes=[t_yb])
                    P.op("vector", lambda e, b=b, yb=yb, r2=r2: e.tensor_scalar(
                        out=r2[:], in0=yb[:, 0:D], scalar1=gates[:, 2 * b:2 * b + 1], scalar2=None, op0=ALU.mult),
                        reads=[t_yb, t_gates[b]], writes=[t_r2])
                    P.op("vector", lambda e, b=b, yb=yb, r2=r2: e.scalar_tensor_tensor(
                        out=r2[:], in0=yb[:, D:2 * D], scalar=gates[:, 2 * b + 1:2 * b + 2], in1=r2[:],
                        op0=ALU.mult, op1=ALU.add), reads=[t_yb, t_gates[b], t_r2], writes=[t_r2])
                    P.op("vector", lambda e, x1b=x1b, r2=r2: e.scalar_tensor_tensor(
                        out=r2[:], in0=x1b[:], scalar=ALPHA, in1=r2[:], op0=ALU.mult, op1=ALU.add),
                        reads=[t_x1b, t_r2], writes=[t_r2])
                    layer_norm_rows((stats, mv, sc, t_stats, t_mv, t_sc), r2, t_r2, lng, lnb, t_ln, r2[:], t_r2, 1)
                    if last_layer:
                        if blk >= 0:
                            P.dma("sync", lambda e, blk=blk, r2=r2: e.dma_start(out=y_out[blk * 128:(blk + 1) * 128, :], in_=r2[:]),
                                  reads=[t_r2], acc=[t_yout])
                    else:
                        P.dma("sync", lambda e, b=b, r2=r2: e.dma_start(out=xa[b * 128:(b + 1) * 128, :], in_=r2[:]),
                              reads=[t_r2], writes=[t_xa[b]])
                P.barrier()
                P.emit()
    return nc


def _consts(cap):
    q = np.arange(128)[:, None]
    kj = np.arange(256)[None, :]
    d = (128 + q - kj).astype(np.float32)
    valid = (d >= 0) & (d < 128)
    dist = np.where(valid, d, np.float32(1e9)).astype(np.float32)
    dist_first = dist.copy()
    dist_first[:, :128] = 1e9
    s = np.arange(128)[:, None]
    t = np.arange(128)[None, :]
    triu = (s <= t).astype(np.float32)
    lstrict = (s < t).astype(np.float32)
    iota = np.tile(np.arange(NE, dtype=np.float32)[None, :], (128, 1))
    tok = np.zeros((128, NBLK, 4), np.int32)
    for b in range(NBLK):
        tl = b * 128 + np.arange(128)
        tok[:, b, 0] = tl
        tok[:, b, 1] = 2 * tl
        tok[:, b, 2] = tl
        tok[:, b, 3] = 2 * tl + 1
    return {
        "c_ident": np.eye(128, dtype=np.float32),
        "c_dist": dist,
        "c_dist_first": dist_first,
        "c_triu": triu,
        "c_lstrict": lstrict,
        "c_iota": iota,
        "c_tokidx": tok.reshape(128, NBLK * 4),
        "c_oob": np.stack([np.full(NE * cap + 256, 256, np.int32),
                           NTOK * 2 + (np.arange(NE * cap + 256) % 128)], axis=1).astype(np.int32),
        "c_pidx": np.stack([NE * cap + np.arange(128), NE * cap + 128 + np.arange(128)], axis=1).astype(np.float32),
    }


FUSED_PLANS = [
    dict(layer=0, inblk=list(range(-2, NOWN)), fullblk=list(range(-1, NOWN))),
    dict(layer=1, inblk=list(range(-1, NOWN)), fullblk=list(range(-1, NOWN))),
    dict(layer=2, inblk=list(range(-1, NOWN)), fullblk=list(range(0, NOWN))),
    dict(layer=3, inblk=list(range(0, NOWN)), fullblk=list(range(0, NOWN))),
]

_NC_CACHE = {}
_IN_NAMES = {}


def run_plans(plans, xs_halo, inputs, cap=CAP):
    key = (repr(plans), cap)
    if key not in _NC_CACHE:
        _NC_CACHE[key] = build_program(plans, cap)
    nc = _NC_CACHE[key]
    cst = _consts(cap)
    f = lambda a: np.ascontiguousarray(a, dtype=np.float32)
    shared = {}
    for p in plans:
        L_ = p["layer"]
        j_ = L_ // 2
        sfx = "_%d" % L_
        if L_ % 2 == 0:
            shared["a_w_in" + sfx] = f(inputs["a_w_in"][j_])
            shared["a_sinks" + sfx] = f(inputs["a_sinks"][j_])
        else:
            shared["b_w_in" + sfx] = f(inputs["b_w_in"][j_])
            shared["b_norm_g" + sfx] = f(inputs["b_norm_g"][j_])
            shared["b_norm_b" + sfx] = f(inputs["b_norm_b"][j_])
            shared["b_w_spatial" + sfx] = f(inputs["b_w_spatial"][j_])
            shared["b_b_spatial" + sfx] = f(inputs["b_b_spatial"][j_]).reshape(12 * 128)
        for nm in ["w_mem_kv", "w_out", "ln_g", "ln_b", "w_router_group", "b_router_group", "w_router_expert",
                   "b_router_expert", "w_gate", "w_up", "w_down"]:
            shared[nm + sfx] = f(inputs[nm][L_])
    mem = f(inputs["mem"])
    in_maps = []
    for c in range(NCORES):
        m = dict(shared)
        m.update(cst)
        if c % CPB != 0:
            m["c_dist_first"] = cst["c_dist"]
        m["x_in"] = xs_halo[c]
        m["mem"] = mem[c // CPB]
        names = _IN_NAMES.get(id(nc))
        if names is not None:
            m = {k: v for k, v in m.items() if k in names}
        in_maps.append(m)
    res = run_bass_kernel_spmd(nc, in_maps, core_ids=list(range(NCORES)))
    return [np.asarray(r["y_out"]) for r in res.results]


def shard_with_halo(x):
    out = []
    for c in range(NCORES):
        bidx, s0 = c // CPB, (c % CPB) * NOWN * 128
        buf = np.zeros((NTOK, D), np.float32)
        lo = max(0, s0 - 256)
        buf[256 - (s0 - lo):] = x[bidx, lo:s0 + NOWN * 128]
        if s0 == 0:
            buf[0:256] = x[bidx, 0:256]
        out.append(buf)
    return out


def kernel(**inputs):
    x = np.ascontiguousarray(inputs["x"], dtype=np.float32)
    ys = run_plans(FUSED_PLANS, shard_with_halo(x), inputs)
    out = np.zeros((2, 8192, D), np.float32)
    for c in range(NCORES):
        out[c // CPB, (c % CPB) * NOWN * 128:(c % CPB + 1) * NOWN * 128] = ys[c]
    return out
```
